# Optimizing a Trainium2 kernel written in Bass

```python
import math
import numpy as np
import jax
import jax.numpy as jnp
from jax import lax

D_MODEL = 1024
BATCH = 8
SEQ = 4096
DEPTH = 4

GRID_W = 64
CTX_LEN = 256
HEAD_DIM = 64
ROPE_THETA = 10000.0
NORM_EPS = 1e-6
F32 = jnp.float32

A_HEADS = 4
A_QK = HEAD_DIM
A_V = 2 * HEAD_DIM
A_QBLOCK = 128
B_HEADS = 4
B_DK = HEAD_DIM // 2
B_DV = HEAD_DIM
B_GATE_RANK = 16
B_GATE_TAU = 16.0
B_CHUNK = 64
C_HEADS = 4
C_DH = HEAD_DIM
NA_ROWS = 8
NA_COLS = 16

A_WIDTH = A_HEADS * A_V
B_WIDTH = B_HEADS * B_DV
C_WIDTH = C_HEADS * C_DH
MIX_WIDTH = A_WIDTH + B_WIDTH + C_WIDTH

IN_SIZES = (A_HEADS * 2 * A_QK, A_HEADS * 2 * A_QK, A_WIDTH,
            B_HEADS * B_DK, B_HEADS * B_DK, B_WIDTH, B_WIDTH, 2 * B_GATE_RANK,
            C_WIDTH, C_WIDTH, C_WIDTH)
IN_WIDTH = sum(IN_SIZES)

N_GROUPS = 4
EXPERTS_PER_GROUP = 8
N_EXPERTS = N_GROUPS * EXPERTS_PER_GROUP
TOP_K = 2
EXPERT_HIDDEN = 512
MOE_BLOCK = 128

kernel_name = 'hybrid_prefix_diffusion_trunk'


def rms_norm(x, g):
    xf = x.astype(F32)
    y = xf * lax.rsqrt(jnp.mean(xf * xf, axis=-1, keepdims=True) + NORM_EPS)
    return (y * g.astype(F32)).astype(x.dtype)


def split_in(p):
    return jnp.split(p, np.cumsum(IN_SIZES)[:-1].tolist(), axis=-1)


def axial_rope_tables(n_tokens, dtype):
    t = jnp.arange(n_tokens)
    row = (t // GRID_W).astype(F32)
    col = (t % GRID_W).astype(F32)
    n_freq = HEAD_DIM // 4
    inv = ROPE_THETA ** (-jnp.arange(n_freq, dtype=F32) / n_freq)
    ang_r = row[:, None] * inv
    ang_c = col[:, None] * inv
    return (jnp.cos(ang_r).astype(dtype), jnp.sin(ang_r).astype(dtype),
            jnp.cos(ang_c).astype(dtype), jnp.sin(ang_c).astype(dtype))


def apply_axial_rope(x, tabs):
    cr, sr, cc, sc = (tb.reshape((tb.shape[0],) + (1,) * (x.ndim - 3) + (tb.shape[1],)) for tb in tabs)
    x1, x2, x3, x4 = jnp.split(x, 4, axis=-1)
    return jnp.concatenate([x1 * cr - x2 * sr, x2 * cr + x1 * sr,
                            x3 * cc - x4 * sc, x4 * cc + x3 * sc], axis=-1)


def diff_attention(q, k, v, qc, kc, vc, lam, lam_init, g_sub, need_ctx):
    B_, S = q.shape[:2]
    L = qc.shape[1]
    scale = A_QK ** -0.5
    k_all = jnp.concatenate([kc, k], axis=1)
    v_all = jnp.concatenate([vc, v], axis=1)

    def attend(qb, kb, vb):
        s = jnp.einsum('bqhmd,bkhmd->bhmqk', qb, kb).astype(F32) * scale
        p = jax.nn.softmax(s, axis=-1)
        d = p[:, :, 0] - lam * p[:, :, 1]
        return jnp.einsum('bhqk,bkhv->bqhv', d.astype(vb.dtype), vb)

    nb = S // A_QBLOCK
    qblocks = jnp.moveaxis(q.reshape((B_, nb, A_QBLOCK) + q.shape[2:]), 1, 0)
    o = lax.map(lambda qb: attend(qb, k_all, v_all), qblocks)
    o = jnp.moveaxis(o, 0, 1).reshape(B_, S, A_HEADS, A_V)
    out = (rms_norm(o, g_sub) * (1.0 - lam_init)).reshape(B_, S, A_WIDTH)
    out_c = None
    if need_ctx:
        oc = attend(qc, kc, vc)
        out_c = (rms_norm(oc, g_sub) * (1.0 - lam_init)).reshape(B_, L, A_WIDTH)
    return out, out_c


def gla_scan(q, k, v, log_a, s0):
    B_, H, T, dk = q.shape
    n = T // B_CHUNK

    def chunks(t):
        return jnp.moveaxis(t.reshape(B_, H, n, B_CHUNK, t.shape[-1]), 2, 0)

    causal = jnp.tril(jnp.ones((B_CHUNK, B_CHUNK), dtype=bool))

    def step(s, inp):
        qc, kc, vc, ac = inp
        b = jnp.cumsum(ac, axis=2)
        rel = jnp.where(causal[:, :, None], b[:, :, :, None, :] - b[:, :, None, :, :], -jnp.inf)
        a_intra = jnp.einsum('bhtsd,bhsd->bhts', qc[:, :, :, None, :] * jnp.exp(rel), kc)
        o = (jnp.einsum('bhts,bhsv->bhtv', a_intra, vc)
             + jnp.einsum('bhtd,bhdv->bhtv', qc * jnp.exp(b), s))
        b_end = b[:, :, -1, :]
        s = (jnp.exp(b_end)[..., None] * s
             + jnp.einsum('bhsd,bhsv->bhdv', kc * jnp.exp(b_end[:, :, None, :] - b), vc))
        return s, o

    s_final, o = lax.scan(step, s0, (chunks(q), chunks(k), chunks(v), chunks(log_a)))
    o = jnp.moveaxis(o, 0, 2).reshape(B_, H, T, v.shape[-1])
    return o, s_final


def gla_mixer(q, k, v, r, glow, qc, kc, vc, rc, glowc, w_dec, b_dec, g_out, need_ctx):
    B_ = q.shape[0]

    def heads(t, d):
        return jnp.transpose(t.reshape(t.shape[0], t.shape[1], -1, d), (0, 2, 1, 3)).astype(F32)

    def decay(gl, direction):
        z = gl[..., direction * B_GATE_RANK:(direction + 1) * B_GATE_RANK] @ w_dec[direction] + b_dec[direction]
        return heads(jax.nn.log_sigmoid(z.astype(F32)) / B_GATE_TAU, B_DK)

    def flip(t):
        return jnp.flip(t, axis=2)

    scale = B_DK ** -0.5
    ql, kl, vl = heads(q, B_DK) * scale, heads(k, B_DK), heads(v, B_DV)
    qx, kx, vx = heads(qc, B_DK) * scale, heads(kc, B_DK), heads(vc, B_DV)
    s0 = jnp.zeros((B_, B_HEADS, B_DK, B_DV), F32)
    ox_f, sx_f = gla_scan(qx, kx, vx, decay(glowc, 0), s0)
    ol_f, _ = gla_scan(ql, kl, vl, decay(glow, 0), sx_f)
    ox_b, sx_b = gla_scan(flip(qx), flip(kx), flip(vx), flip(decay(glowc, 1)), s0)
    ol_b, _ = gla_scan(flip(ql), flip(kl), flip(vl), flip(decay(glow, 1)), sx_b)

    def readout(o, gate):
        o = jnp.transpose(rms_norm(o, g_out), (0, 2, 1, 3)).reshape(gate.shape[0], gate.shape[1], B_WIDTH)
        return (o * jax.nn.silu(gate.astype(F32))).astype(gate.dtype)

    out = readout(ol_f + flip(ol_b), r)
    out_c = readout(ox_f + flip(ox_b), rc) if need_ctx else None
    return out, out_c


def neighborhood_attention(q, k, v, qc, kc, vc, rpb, need_ctx):
    B_, S, H, dh = q.shape
    L = qc.shape[1]
    rows = S // GRID_W
    wr = min(NA_ROWS, rows)
    scale = dh ** -0.5

    def grid(t):
        return jnp.transpose(t.reshape(B_, rows, GRID_W, H, dh), (0, 3, 1, 2, 4))

    qg, kg, vg = grid(q), grid(k), grid(v)
    r = jnp.arange(rows)
    row_idx = jnp.clip(r - wr // 2, 0, rows - wr)[:, None] + jnp.arange(wr)
    cq = jnp.arange(GRID_W)
    col_start = jnp.clip(cq - NA_COLS // 2, 0, GRID_W - NA_COLS)
    col_mask = (cq[None, :] >= col_start[:, None]) & (cq[None, :] < col_start[:, None] + NA_COLS)
    k_rows = kg[:, :, row_idx]
    v_rows = vg[:, :, row_idx]
    s_nb = jnp.einsum('bhrqd,bhrwkd->bhrqwk', qg, k_rows).astype(F32) * scale
    dr = row_idx - r[:, None] + (NA_ROWS - 1)
    dc = jnp.clip(cq[None, :] - cq[:, None], -(NA_COLS - 1), NA_COLS - 1) + (NA_COLS - 1)
    bias = rpb[:, dr[:, None, :, None], dc[None, :, None, :]].astype(F32)
    s_nb = jnp.where(col_mask[:, None, :], s_nb + bias, -jnp.inf)
    s_c = jnp.einsum('bhrqd,bchd->bhrqc', qg, kc).astype(F32) * scale
    n_nb = wr * GRID_W
    p = jax.nn.softmax(jnp.concatenate([s_nb.reshape(B_, H, rows, GRID_W, n_nb), s_c], axis=-1), axis=-1)
    p_nb = p[..., :n_nb].reshape(s_nb.shape).astype(v.dtype)
    p_c = p[..., n_nb:].astype(v.dtype)
    o = (jnp.einsum('bhrqwk,bhrwkd->bhrqd', p_nb, v_rows)
         + jnp.einsum('bhrqc,bchd->bhrqd', p_c, vc))
    out = jnp.transpose(o, (0, 2, 3, 1, 4)).reshape(B_, S, H * dh)
    out_c = None
    if need_ctx:
        sc = jnp.einsum('bqhd,bkhd->bhqk', qc, kc).astype(F32) * scale
        out_c = jnp.einsum('bhqk,bkhd->bqhd', jax.nn.softmax(sc, axis=-1).astype(vc.dtype), vc).reshape(B_, L, H * dh)
    return out, out_c


def hier_moe(h, w_rg, b_rg, w_re, b_re, w_up, w_down):
    n_tok, D = h.shape
    n_assign = n_tok * TOP_K
    n_blocks = -(-(n_assign + N_EXPERTS * (MOE_BLOCK - 1)) // MOE_BLOCK)
    logit_g = (h @ w_rg + b_rg).astype(F32)
    grp = jnp.argmax(logit_g, axis=-1)
    p_grp = jnp.take_along_axis(jax.nn.softmax(logit_g, axis=-1), grp[:, None], axis=1)
    logit_e = (h @ w_re + b_re).astype(F32).reshape(n_tok, N_GROUPS, EXPERTS_PER_GROUP)
    logit_e = jnp.take_along_axis(logit_e, grp[:, None, None], axis=1)[:, 0]
    top_v, top_i = lax.top_k(logit_e, TOP_K)
    gate = jax.nn.softmax(top_v, axis=-1) * p_grp
    expert = (grp[:, None] * EXPERTS_PER_GROUP + top_i).reshape(-1)
    order = jnp.argsort(expert)
    e_sorted = expert[order]
    tok_sorted = order // TOP_K
    counts = jnp.bincount(expert, length=N_EXPERTS)
    padded = (counts + MOE_BLOCK - 1) // MOE_BLOCK * MOE_BLOCK
    start = jnp.cumsum(counts) - counts
    pad_end = jnp.cumsum(padded)
    pad_start = pad_end - padded
    dest = pad_start[e_sorted] + jnp.arange(n_assign) - start[e_sorted]
    buf = jnp.zeros((n_blocks * MOE_BLOCK, D), h.dtype).at[dest].set(h[tok_sorted])
    blk_expert = jnp.minimum(jnp.searchsorted(pad_end, jnp.arange(n_blocks) * MOE_BLOCK, side='right'), N_EXPERTS - 1)

    def expert_block(args):
        xb, e = args
        g, u = jnp.split(xb @ w_up[e], 2, axis=-1)
        return (jax.nn.silu(g) * u) @ w_down[e]

    y = lax.map(expert_block, (buf.reshape(n_blocks, MOE_BLOCK, D), blk_expert)).reshape(-1, D)
    y = y[dest] * gate.reshape(-1)[order][:, None].astype(h.dtype)
    return jax.ops.segment_sum(y, tok_sorted, num_segments=n_tok)


def hybrid_layer(x, xc, c_act, cc_act, rope, layer_idx, need_ctx,
                 w_mod, b_mod, g1, g2, w_in, w_out, lam, g_sub,
                 w_dec, b_dec, g_gla, rpb, w_rg, b_rg, w_re, b_re, w_up, w_down):
    B_, S, D = x.shape
    L = xc.shape[1]
    mod = (c_act @ w_mod + b_mod).reshape(B_, 6, 1, D)
    modc = (cc_act @ w_mod + b_mod).reshape(6, D)
    h = rms_norm(x, g1) * (1 + mod[:, 1]) + mod[:, 0]
    hc = rms_norm(xc, g1) * (1 + modc[1]) + modc[0]
    qa, ka, va, qb, kb, vb, rb, gb, qn, kn, vn = split_in(h @ w_in)
    qa_c, ka_c, va_c, qb_c, kb_c, vb_c, rb_c, gb_c, qn_c, kn_c, vn_c = split_in(hc @ w_in)

    lam_init = 0.8 - 0.6 * math.exp(-0.3 * layer_idx)
    lam_f = lam.astype(F32)
    lam_val = jnp.exp(jnp.sum(lam_f[0] * lam_f[1])) - jnp.exp(jnp.sum(lam_f[2] * lam_f[3])) + lam_init

    def a_qk(t):
        return t.reshape(t.shape[0], t.shape[1], A_HEADS, 2, A_QK)

    ya, ya_c = diff_attention(apply_axial_rope(a_qk(qa), rope), apply_axial_rope(a_qk(ka), rope),
                              va.reshape(B_, S, A_HEADS, A_V), a_qk(qa_c), a_qk(ka_c),
                              va_c.reshape(B_, L, A_HEADS, A_V), lam_val, lam_init, g_sub, need_ctx)
    yb, yb_c = gla_mixer(qb, kb, vb, rb, gb, qb_c, kb_c, vb_c, rb_c, gb_c, w_dec, b_dec, g_gla, need_ctx)

    def c_heads(t):
        return t.reshape(t.shape[0], t.shape[1], C_HEADS, C_DH)

    yn, yn_c = neighborhood_attention(c_heads(qn), c_heads(kn), c_heads(vn),
                                      c_heads(qn_c), c_heads(kn_c), c_heads(vn_c), rpb, need_ctx)
    x = x + mod[:, 2] * (jnp.concatenate([ya, yb, yn], axis=-1) @ w_out)
    h2 = rms_norm(x, g2) * (1 + mod[:, 4]) + mod[:, 3]
    if need_ctx:
        xc = xc + modc[2] * (jnp.concatenate([ya_c, yb_c, yn_c], axis=-1) @ w_out)
        h2c = rms_norm(xc, g2) * (1 + modc[4]) + modc[3]
        f = hier_moe(jnp.concatenate([h2.reshape(-1, D), h2c.reshape(-1, D)], axis=0),
                     w_rg, b_rg, w_re, b_re, w_up, w_down)
        x = x + mod[:, 5] * f[:B_ * S].reshape(B_, S, D)
        xc = xc + modc[5] * f[B_ * S:].reshape(B_, L, D)
    else:
        f = hier_moe(h2.reshape(-1, D), w_rg, b_rg, w_re, b_re, w_up, w_down)
        x = x + mod[:, 5] * f.reshape(B_, S, D)
    return x, xc


def setup_inputs(seed: int = 0) -> dict:
    key = jax.random.key(seed)
    ks = jax.random.split(key, 23)
    D = D_MODEL

    def nrm(k, shape, s):
        return jax.random.normal(k, shape, F32) * s

    return {
        'x': nrm(ks[0], (BATCH, SEQ, D), 1.0),
        'c': nrm(ks[1], (BATCH, D), 1.0),
        'ctx': nrm(ks[2], (BATCH, CTX_LEN, D), 1.0),
        'c_ctx': nrm(ks[3], (D,), 1.0),
        'w_mod': nrm(ks[4], (DEPTH, D, 6 * D), 0.5 * D ** -0.5),
        'b_mod': nrm(ks[5], (DEPTH, 6 * D), 0.02),
        'norm1_g': 1.0 + nrm(ks[6], (DEPTH, D), 0.02),
        'norm2_g': 1.0 + nrm(ks[7], (DEPTH, D), 0.02),
        'w_in': nrm(ks[8], (DEPTH, D, IN_WIDTH), D ** -0.5),
        'w_out': nrm(ks[9], (DEPTH, MIX_WIDTH, D), MIX_WIDTH ** -0.5),
        'diff_lambda': nrm(ks[10], (DEPTH, 4, A_QK), 0.1),
        'diff_sub_g': 1.0 + nrm(ks[11], (DEPTH, A_V), 0.02),
        'gla_w_decay': nrm(ks[12], (DEPTH, 2, B_GATE_RANK, B_HEADS * B_DK), B_GATE_RANK ** -0.5),
        'gla_b_decay': nrm(ks[13], (DEPTH, 2, B_HEADS * B_DK), 0.1),
        'gla_norm_g': 1.0 + nrm(ks[14], (DEPTH, B_DV), 0.02),
        'na_rel_bias': nrm(ks[15], (DEPTH, C_HEADS, 2 * NA_ROWS - 1, 2 * NA_COLS - 1), 0.02),
        'w_router_group': nrm(ks[16], (DEPTH, D, N_GROUPS), D ** -0.5),
        'b_router_group': nrm(ks[17], (DEPTH, N_GROUPS), 0.01),
        'w_router_expert': nrm(ks[18], (DEPTH, D, N_EXPERTS), D ** -0.5),
        'b_router_expert': nrm(ks[19], (DEPTH, N_EXPERTS), 0.01),
        'w_expert_up': nrm(ks[20], (DEPTH, N_EXPERTS, D, 2 * EXPERT_HIDDEN), D ** -0.5),
        'w_expert_down': nrm(ks[21], (DEPTH, N_EXPERTS, EXPERT_HIDDEN, D), EXPERT_HIDDEN ** -0.5),
        'final_g': 1.0 + nrm(ks[22], (D,), 0.02),
    }


def reference(x, c, ctx, c_ctx, w_mod, b_mod, norm1_g, norm2_g, w_in, w_out,
              diff_lambda, diff_sub_g, gla_w_decay, gla_b_decay, gla_norm_g, na_rel_bias,
              w_router_group, b_router_group, w_router_expert, b_router_expert,
              w_expert_up, w_expert_down, final_g):
    rope = axial_rope_tables(x.shape[1], x.dtype)
    c_act = jax.nn.silu(c)
    cc_act = jax.nn.silu(c_ctx)
    xc = ctx
    for l in range(DEPTH):
        x, xc = hybrid_layer(x, xc, c_act, cc_act, rope, l, l < DEPTH - 1,
                             w_mod[l], b_mod[l], norm1_g[l], norm2_g[l], w_in[l], w_out[l],
                             diff_lambda[l], diff_sub_g[l], gla_w_decay[l], gla_b_decay[l],
                             gla_norm_g[l], na_rel_bias[l], w_router_group[l], b_router_group[l],
                             w_router_expert[l], b_router_expert[l], w_expert_up[l], w_expert_down[l])
    return rms_norm(x, final_g)
```

```python
import contextlib
import math
import numpy as np
import ml_dtypes
import concourse.bass as bass
import concourse.mybir as mybir
from concourse.bass_utils import run_bass_kernel_spmd

F32 = mybir.dt.float32
BF16 = mybir.dt.bfloat16
ALU = mybir.AluOpType
AF = mybir.ActivationFunctionType
AX = mybir.AxisListType

D = 1024
SEQ = 4096
CTX = 256
T = SEQ + CTX
NT = T // 128
DEPTH = 4
INW = 3104
NE = 32
EPS = 1e-6
NEG = -30000.0

ENGS = ("pe", "act", "dve", "pool", "sp")
SEM_WRAP = 40000


class _Op:
    __slots__ = ("eng", "fn", "deps", "signalled", "sigidx", "dma", "pos")

    def __init__(self, eng, fn):
        self.eng = eng
        self.fn = fn
        self.deps = []
        self.signalled = False
        self.sigidx = None
        self.dma = None


class Prog:
    def __init__(self, nc):
        self.nc = nc
        self.stack = contextlib.ExitStack()
        self.sigcount = {e: 0 for e in ENGS}
        self.eng_sems = {e: [] for e in ENGS}
        self.dma_keys = {}
        self.free_dma = {}
        self.retired = []
        self.waited_eng = {e: {s: -1 for s in ENGS} for e in ENGS}
        self.waited_dma = {e: {} for e in ENGS}
        self.nsem = 0
        self.bar_sem = self._new_sem("bar")
        self.bar_count = 0
        self.npos = 0
        self._reset_phase()

    def _reset_phase(self):
        self.ops = {e: [] for e in ENGS}
        self.last_writer = {}
        self.readers = {}

    def _new_sem(self, name):
        self.nsem += 1
        return self.stack.enter_context(self.nc.semaphore(f"{name}_{self.nsem}"))

    def _eng_sem(self, eng, k):
        lst = self.eng_sems[eng]
        while len(lst) <= k:
            lst.append(self._new_sem(f"s_{eng}"))
        return lst[k]

    def _track(self, op, reads, writes):
        deps = []
        for r in reads:
            w = self.last_writer.get(r)
            if w is not None:
                deps.append(w)
        for w_ in writes:
            w = self.last_writer.get(w_)
            if w is not None:
                deps.append(w)
            deps.extend(self.readers.get(w_, ()))
        for r in reads:
            self.readers.setdefault(r, []).append(op)
        for w_ in writes:
            self.last_writer[w_] = op
            self.readers[w_] = []
        seen = set()
        latest = {}
        for d in deps:
            if d is op or id(d) in seen:
                continue
            seen.add(id(d))
            if d.dma is None:
                if d.eng == "pe" and op.eng == "pe":
                    continue
                cur = latest.get(d.eng)
                if cur is None or d.pos > cur.pos:
                    latest[d.eng] = d
            else:
                op.deps.append(d)
        for d in latest.values():
            d.signalled = True
            op.deps.append(d)

    def op(self, eng, fn, reads=(), writes=()):
        o = _Op(eng, fn)
        o.pos = self.npos
        self.npos += 1
        self._track(o, reads, writes)
        self.ops[eng].append(o)
        return o

    def dma(self, q, out, in_, reads=(), writes=(), key=None):
        if key is None:
            key = ("auto", tuple(writes), tuple(reads))
        ent = self.dma_keys.get(key)
        if ent is None or ent[1] + 16 > SEM_WRAP:
            if ent is not None:
                self.retired.append(ent)
            ent = None
            pool_ = self.free_dma.setdefault(q, [])
            while pool_:
                cand = pool_.pop()
                if cand[1] + 4096 <= SEM_WRAP:
                    ent = cand
                    break
            if ent is None:
                ent = [self._new_sem("d"), 0, q]
            self.dma_keys[key] = ent
        ent[1] += 16
        sem, val = ent[0], ent[1]

        def fn(e, out=out, in_=in_, sem=sem):
            return e.dma_start(out=out, in_=in_).then_inc(sem, 16)

        o = _Op(q, fn)
        o.dma = (sem, val)
        o.pos = self.npos
        self.npos += 1
        self._track(o, reads, writes)
        self.ops[q].append(o)
        return o

    def end_phase(self):
        nc = self.nc
        last = {}
        for e in ENGS:
            for o in reversed(self.ops[e]):
                if o.dma is None:
                    o.signalled = True
                    last[e] = o
                    break
        for e in ENGS:
            for o in self.ops[e]:
                if o.dma is None and o.signalled:
                    o.sigidx = self.sigcount[e]
                    self.sigcount[e] += 1
        self.bar_count += 1
        bar_val = self.bar_count
        dma_final = [(ent[0], ent[1]) for ent in list(self.dma_keys.values()) + self.retired if ent[1] > 0]
        self.retired = []
        for ent in self.dma_keys.values():
            self.free_dma.setdefault(ent[2], []).append(ent)
        for q_ in self.free_dma:
            self.free_dma[q_].sort(key=lambda t: -t[1])
        self.dma_keys = {}

        def emit_waits(e, eng_name, deps):
            for d in deps:
                if d.dma is not None:
                    sem, val = d.dma
                    w = self.waited_dma[eng_name]
                    if w.get(id(sem), 0) >= val:
                        continue
                    w[id(sem)] = val
                    e.wait_ge(sem, val)
                else:
                    w = self.waited_eng[eng_name]
                    if w[d.eng] >= d.sigidx:
                        continue
                    w[d.eng] = d.sigidx
                    e.wait_ge(self._eng_sem(d.eng, d.sigidx // SEM_WRAP), d.sigidx % SEM_WRAP + 1)

        def emit(e, eng_name):
            for o in self.ops[eng_name]:
                emit_waits(e, eng_name, o.deps)
                ins = o.fn(e)
                if o.dma is None and o.signalled:
                    ins.then_inc(self._eng_sem(eng_name, o.sigidx // SEM_WRAP), 1)
            if eng_name == "sp":
                for en, o in last.items():
                    emit_waits(e, "sp", [o])
                for sem, val in dma_final:
                    w = self.waited_dma["sp"]
                    if w.get(id(sem), 0) >= val:
                        continue
                    w[id(sem)] = val
                    e.wait_ge(sem, val)
                e.sem_inc(self.bar_sem, 1)
            else:
                e.wait_ge(self.bar_sem, bar_val)

        for en in ENGS:
            if self.sigcount[en] > 0:
                self._eng_sem(en, (self.sigcount[en] - 1) // SEM_WRAP)

        with nc.Block() as block:
            @block.tensor
            def _(e):
                emit(e, "pe")

            @block.scalar
            def _(e):
                emit(e, "act")

            @block.vector
            def _(e):
                emit(e, "dve")

            @block.gpsimd
            def _(e):
                emit(e, "pool")

            @block.sync
            def _(e):
                emit(e, "sp")
        self._reset_phase()

    def close(self):
        self.stack.close()


def na_plan():
    pats = {}
    plist = []
    plan = []
    kl = np.arange(128)
    ql = np.arange(128)
    for j in range(32):
        rq = 2 * j + ql // 64
        cq = ql % 64
        rs = np.clip(rq - 4, 0, 56)
        cs = np.clip(cq - 8, 0, 48)
        kt_lo = int(rs.min()) // 2
        kt_hi = (int(rs.max()) + 7) // 2
        lst = []
        for kt in range(kt_lo, kt_hi + 1):
            rk = (2 * kt + kl // 64)[:, None]
            ck = (kl % 64)[:, None]
            valid = (rk >= rs[None, :]) & (rk < rs[None, :] + 8) & (ck >= cs[None, :]) & (ck < cs[None, :] + 16)
            if not valid.any():
                continue
            dr = np.clip(rk - rq[None, :] + 7, 0, 14)
            dc = np.clip(ck - cq[None, :], -15, 15) + 15
            key = (kt - j, valid.tobytes())
            if key not in pats:
                pats[key] = len(plist)
                plist.append((valid, dr, dc))
            lst.append((kt, pats[key]))
        plan.append(lst)
    return plan, plist


_NA_PLAN, _NA_PATS = na_plan()
NPAT = len(_NA_PATS)


def host_consts():
    c = {}
    c["identb"] = np.eye(128, dtype=np.float32).astype(ml_dtypes.bfloat16)
    t = np.arange(SEQ)
    row = (t // 64).astype(np.float32)
    col = (t % 64).astype(np.float32)
    inv = (10000.0 ** (-np.arange(16, dtype=np.float32) / 16)).astype(np.float32)
    ang = np.concatenate([row[:, None] * inv, col[:, None] * inv], axis=1).astype(np.float32)
    c["ropeC"] = np.ascontiguousarray(np.cos(ang).astype(np.float32).reshape(32, 128, 32).transpose(1, 0, 2))
    c["ropeS"] = np.ascontiguousarray(np.sin(ang).astype(np.float32).reshape(32, 128, 32).transpose(1, 0, 2))
    s = np.arange(128)[:, None]
    tt = np.arange(128)[None, :]
    same = (s // 64) == (tt // 64)
    glm = np.stack([same & (s <= tt), same & (s > tt), same & (s >= tt), same & (s < tt)]).astype(np.float32)
    c["glm"] = np.ascontiguousarray(glm.transpose(1, 0, 2))
    c["chunkind"] = (np.arange(128)[:, None] // 64 == np.arange(2)[None, :]).astype(np.float32)
    hm4 = (np.arange(128)[:, None, None] // 32 == np.arange(4)[None, :, None]) & np.ones((1, 1, 128), bool)
    c["hm4"] = hm4.astype(np.float32).astype(ml_dtypes.bfloat16)
    c["bd"] = (np.arange(128)[:, None] // 32 == np.arange(256)[None, :] // 64).astype(np.float32)
    return c


def na_bias_host(rpb):
    L = rpb.shape[0]
    out = np.empty((L, 128, NPAT, 4, 128), np.float32)
    for p, (valid, dr, dc) in enumerate(_NA_PATS):
        g = rpb[:, :, dr, dc]
        g = np.where(valid[None, None], g, np.float32(NEG))
        out[:, :, p, :, :] = g[:, [0, 2, 1, 3]].transpose(0, 2, 1, 3)
    return out


class _Stop(Exception):
    pass


def build_program(n_layers=DEPTH, debug=False, stop_after=None):
    nc = bass.Bass("TRN2", target_bir_lowering=False)
    P = Prog(nc)

    def din(name, shape, dt=F32):
        return nc.dram_tensor(name, list(shape), dt, kind="ExternalInput").ap()

    def dscr(name, shape, dt):
        return nc.dram_tensor(name, list(shape), dt, kind=("ExternalOutput" if debug else "Internal")).ap()

    DL = n_layers
    x_in = din("x", [SEQ, D])
    c_in = din("c", [D])
    ctx_in = din("ctx", [CTX, D])
    cctx_in = din("c_ctx", [D])
    w_mod = din("w_mod", [DL, D, 6 * D])
    b_mod = din("b_mod", [DL, 6 * D])
    norm1_g = din("norm1_g", [DL, D])
    norm2_g = din("norm2_g", [DL, D])
    w_in = din("w_in", [DL, D, INW])
    w_out = din("w_out", [DL, D, D])
    diff_lambda = din("diff_lambda", [DL, 4, 64])
    diff_sub_g = din("diff_sub_g", [DL, 128])
    gla_w_decay = din("gla_w_decay", [DL, 2, 16, 128])
    gla_b_decay = din("gla_b_decay", [DL, 2, 128])
    gla_norm_g = din("gla_norm_g", [DL, 64])
    w_rg = din("w_router_group", [DL, D, 4])
    b_rg = din("b_router_group", [DL, 4])
    w_re = din("w_router_expert", [DL, D, 32])
    b_re = din("b_router_expert", [DL, 32])
    NED = NE if stop_after in (None, "moe") else 1
    w_up = din("w_expert_up", [DL, NED, D, D])
    w_down = din("w_expert_down", [DL, NED, 512, D])
    final_g = din("final_g", [D])
    identb_d = din("identb", [128, 128], BF16)
    ropeC_d = din("ropeC", [128, 32, 32])
    ropeS_d = din("ropeS", [128, 32, 32])
    glm_d = din("glm", [128, 4, 128])
    chunkind_d = din("chunkind", [128, 2])
    hm4_d = din("hm4", [128, 4, 128], BF16)
    bd_d = din("bd", [128, 256])
    bmC_d = din("bmC", [DL, 128, NPAT, 4, 128])

    out_d = nc.dram_tensor("out", [SEQ, D], F32, kind="ExternalOutput").ap()

    xs = dscr("xs", [T, D], F32)
    modrow = dscr("modrow", [2, 6, D], F32)
    QAT = dscr("QAT", [512, T], BF16)
    KAT = dscr("KAT", [512, T], BF16)
    VA = dscr("VA", [T, 516], BF16)
    BTOK = dscr("BTOK", [T, 768], F32)
    GBT = dscr("GBT", [32, T], F32)
    QNT = dscr("QNT", [256, T], BF16)
    KNT = dscr("KNT", [256, T], BF16)
    VN = dscr("VN", [T, 260], BF16)
    YCAT = dscr("YCAT", [T, D], BF16)
    H2T = dscr("H2T", [D, T], BF16)

    def x_rows(l, i):
        if l == 0:
            if i < 2:
                return ctx_in[i * 128:(i + 1) * 128, :]
            return x_in[(i - 2) * 128:(i - 1) * 128, :]
        return xs[i * 128:(i + 1) * 128, :]

    with contextlib.suppress(_Stop), contextlib.ExitStack() as gs:
        def gsb(name, shape, dt):
            return gs.enter_context(nc.sbuf_tensor(name, list(shape), dt))

        identb = gsb("identb_sb", [128, 128], BF16)
        cact2 = gsb("cact2", [128, 8, 2], F32)
        G_all = gsb("G_all", [128, NT, 32], F32)
        ones_b = gsb("ones_b", [1, 128], BF16)

        with contextlib.ExitStack() as ps_:
            craw = ps_.enter_context(nc.sbuf_tensor("craw", [128, 2, 8], F32))
            P.dma("sp", identb[:], identb_d[:, :], writes=["identb"])
            P.dma("sp", craw[:, 0, :], c_in.rearrange("(p k) -> p k", k=8), writes=["craw0"])
            P.dma("sp", craw[:, 1, :], cctx_in.rearrange("(p k) -> p k", k=8), writes=["craw1"])
            P.op("act", lambda e: e.activation(out=cact2[:].rearrange("p k t -> p t k"), in_=craw[:], func=AF.Silu),
                 reads=["craw0", "craw1"], writes=["cact2"])
            P.op("dve", lambda e: e.memset(ones_b[:], 1.0), writes=["ones_b"])
            P.end_phase()

        for l in range(n_layers):
            need_ctx = l < DEPTH - 1
            last_layer = l == DEPTH - 1
            lam_init = 0.8 - 0.6 * math.exp(-0.3 * l)
            tiles_q = list(range(NT)) if need_ctx else list(range(2, NT))

            with contextlib.ExitStack() as ph:
                def sb(name, shape, dt):
                    return ph.enter_context(nc.sbuf_tensor(f"{name}_m{l}", list(shape), dt))

                def pp(name, shape, dt):
                    return ph.enter_context(nc.psum_tensor(f"{name}_m{l}", list(shape), dt))

                wm = sb("wm", [128, 2, 8, 512], F32)
                bias2 = sb("bias2", [2, 6 * D], F32)
                g12 = sb("g12", [2, 2, D], F32)
                res = sb("res", [2, 2, 512], F32)
                pm_full = pp("pm", [128, 2, 512], F32)
                pm = pm_full[0:2]
                P.dma("sp", bias2[:], b_mod[l].partition_broadcast(2), writes=["bias2"])
                P.dma("sp", g12[:, 0, :], norm1_g[l].partition_broadcast(2), writes=["g12a"])
                P.dma("sp", g12[:, 1, :], norm2_g[l].partition_broadcast(2), writes=["g12b"])
                wsrc = w_mod[l].rearrange("(p k) n -> p k n", k=8)
                for j in range(12):
                    v, half = j // 2, j % 2
                    s_ = j % 2
                    P.dma("sp", wm[:, s_], wsrc[:, :, j * 512:(j + 1) * 512], writes=[f"wm{s_}"])
                    for kc in range(8):
                        P.op("pe", lambda e, kc=kc, s_=s_: e.matmul(pm[:, s_, :], cact2[:, kc, :], wm[:, s_, kc, :],
                                                                   start=(kc == 0), stop=(kc == 7)),
                             reads=[f"wm{s_}", "cact2"], writes=[f"pm{s_}"])
                    P.op("dve", lambda e, s_=s_, j=j: e.tensor_tensor(out=res[:, s_, :], in0=pm[:, s_, :],
                                                                       in1=bias2[:, j * 512:(j + 1) * 512], op=ALU.add),
                         reads=[f"pm{s_}", "bias2"], writes=[f"res{s_}"])
                    if v in (1, 4):
                        gi = 0 if v == 1 else 1
                        P.op("dve", lambda e, s_=s_, gi=gi, half=half: e.scalar_tensor_tensor(
                            out=res[:, s_, :], in0=res[:, s_, :], scalar=1.0, in1=g12[:, gi, half * 512:(half + 1) * 512],
                            op0=ALU.add, op1=ALU.mult), reads=[f"res{s_}", "g12a", "g12b"], writes=[f"res{s_}"])
                    P.dma("sp", modrow[:, v, half * 512:(half + 1) * 512], res[:, s_, :], reads=[f"res{s_}"],
                          key=("modst", s_))
                P.end_phase()
                if stop_after == "mod" and l == n_layers - 1:
                    raise _Stop()

            with contextlib.ExitStack() as ph:
                def sb(name, shape, dt):
                    return ph.enter_context(nc.sbuf_tensor(f"{name}_p{l}", list(shape), dt))

                def pp(name, shape, dt):
                    return ph.enter_context(nc.psum_tensor(f"{name}_p{l}", list(shape), dt))

                win = sb("win", [128, 8, INW], BF16)
                A1 = sb("A1", [128, 2, D], F32)
                S1 = sb("S1", [128, 2, D], F32)
                ropeC = sb("ropeC", [128, 32, 32], F32)
                ropeS = sb("ropeS", [128, 32, 32], F32)
                xt = sb("xt", [128, 2, D], F32)
                junk = sb("junk", [128, D], F32)
                ss = sb("ss", [128, 2], F32)
                rstd = sb("rstd", [128, 2], F32)
                htmp = sb("htmp", [128, D], F32)
                hb = sb("hb", [128, D], BF16)
                hT = sb("hT", [128, 2, 8, 128], BF16)
                qk = sb("qk", [128, 2, 512], BF16)
                t1 = sb("t1", [128, 256], F32)
                t2 = sb("t2", [128, 256], F32)
                qkT = sb("qkT", [128, 2, 4, 128], BF16)
                vaug = sb("vaug", [128, 2, 4, 129], BF16)
                btok = sb("btok", [128, 2, 768], F32)
                gbt = sb("gbt", [32, 2, 128], F32)
                nT = sb("nT", [128, 2, 4, 128], BF16)
                vnaug = sb("vnaug", [128, 2, 4, 65], BF16)
                pT = pp("pT", [128, 8, 128], BF16)
                pj = pp("pj", [128, 2, 512], F32)
                pqT = pp("pqT", [128, 2, 8, 128], BF16)
                pn = pp("pn", [128, 4, 128], F32)
                pg_full = pp("pg", [128, 512], F32)
                pg = pg_full[0:32, 0:128]

                for kc in range(8):
                    P.dma("pool", win[:, kc, :], w_in[l, kc * 128:(kc + 1) * 128, :], writes=[f"win{kc}"])
                winR = [f"win{kc}" for kc in range(8)]
                for t_ in range(2):
                    P.dma("sp", A1[:, t_, :], modrow[t_, 1].partition_broadcast(128), writes=["A1"], key=("A1", t_))
                    P.dma("sp", S1[:, t_, :], modrow[t_, 0].partition_broadcast(128), writes=["S1"], key=("S1", t_))
                P.dma("sp", ropeC[:], ropeC_d[:, :, :], writes=["ropeC"])
                P.dma("sp", ropeS[:], ropeS_d[:, :, :], writes=["ropeS"])
                P.op("dve", lambda e: e.memset(vaug[:], 1.0), writes=["vaug0", "vaug1"])
                P.op("dve", lambda e: e.memset(vnaug[:], 1.0), writes=["vnaug0", "vnaug1"])

                pjcnt = 0

                def stage1(i):
                    s_ = i % 2
                    lat = 0 if i >= 2 else 1
                    P.dma("sp", xt[:, s_, :], x_rows(l, i), writes=[f"xt{s_}"])
                    P.op("act", lambda e, s_=s_: e.activation(out=junk[:], in_=xt[:, s_, :], func=AF.Square,
                                                               accum_out=ss[:, s_:s_ + 1]),
                         reads=[f"xt{s_}"], writes=["junk", f"ss{s_}"])
                    P.op("act", lambda e, s_=s_: e.activation(out=rstd[:, s_:s_ + 1], in_=ss[:, s_:s_ + 1], func=AF.Ln,
                                                               scale=1.0 / D, bias=EPS),
                         reads=[f"ss{s_}"], writes=[f"rstd{s_}"])
                    P.op("act", lambda e, s_=s_: e.activation(out=rstd[:, s_:s_ + 1], in_=rstd[:, s_:s_ + 1], func=AF.Exp,
                                                               scale=-0.5),
                         reads=[f"rstd{s_}"], writes=[f"rstd{s_}"])
                    P.op("dve", lambda e, s_=s_, lat=lat: e.scalar_tensor_tensor(
                        out=htmp[:], in0=xt[:, s_, :], scalar=rstd[:, s_:s_ + 1], in1=A1[:, lat, :], op0=ALU.mult, op1=ALU.mult),
                        reads=[f"xt{s_}", f"rstd{s_}", "A1"], writes=["htmp"])
                    P.op("dve", lambda e, lat=lat: e.tensor_tensor(out=hb[:], in0=htmp[:], in1=S1[:, lat, :], op=ALU.add),
                         reads=["htmp", "S1"], writes=["hb"])
                    for kc in range(8):
                        P.op("pe", lambda e, kc=kc: e.transpose(out=pT[:, kc, :], in_=hb[:, kc * 128:(kc + 1) * 128],
                                                                identity=identb[:]),
                             reads=["hb", "identb"], writes=["pT"])
                    P.op("act", lambda e, s_=s_: e.copy(out=hT[:, s_], in_=pT[:]), reads=["pT"], writes=[f"hT{s_}"])

                def stage2(i):
                    s_ = i % 2
                    hTk = f"hT{s_}"

                    def proj(c0, n, i=i, s_=s_, hTk=hTk):
                        nonlocal pjcnt
                        b_ = pjcnt % 2
                        pjcnt += 1
                        for kc in range(8):
                            P.op("pe", lambda e, kc=kc, b_=b_: e.matmul(pj[:, b_, 0:n], hT[:, s_, kc, :], win[:, kc, c0:c0 + n],
                                                                       start=(kc == 0), stop=(kc == 7)),
                                 reads=[hTk] + winR, writes=[f"pj{b_}"])
                        return b_

                    for qi, (c0, dst) in enumerate(((0, QAT), (512, KAT))):
                        b_ = proj(c0, 512)
                        if i >= 2:
                            j = i - 2
                            src = pj[:, b_, :].rearrange("p (h r w d) -> p h r w d", h=8, r=2, w=2, d=16)
                            dstv = qk[:, qi, :].rearrange("p (h r w d) -> p h r w d", h=8, r=2, w=2, d=16)
                            Cb = ropeC[:, j, :].rearrange("p (r d) -> p r d", r=2).unsqueeze(1).broadcast_to([128, 8, 2, 16])
                            Sb = ropeS[:, j, :].rearrange("p (r d) -> p r d", r=2).unsqueeze(1).broadcast_to([128, 8, 2, 16])
                            t1v = t1[:].rearrange("p (h r d) -> p h r d", h=8, r=2, d=16)
                            t2v = t2[:].rearrange("p (h r d) -> p h r d", h=8, r=2, d=16)
                            xa = src[:, :, :, 0, :]
                            xb = src[:, :, :, 1, :]
                            rk = [f"pj{b_}", "ropeC", "ropeS"]
                            P.op("dve", lambda e, xa=xa, Cb=Cb, t1v=t1v: e.tensor_tensor(out=t1v, in0=xa, in1=Cb, op=ALU.mult),
                                 reads=rk, writes=["t1"])
                            P.op("dve", lambda e, xb=xb, Sb=Sb, t2v=t2v: e.tensor_tensor(out=t2v, in0=xb, in1=Sb, op=ALU.mult),
                                 reads=rk, writes=["t2"])
                            P.op("dve", lambda e, dstv=dstv, t1v=t1v, t2v=t2v: e.tensor_tensor(
                                out=dstv[:, :, :, 0, :], in0=t1v, in1=t2v, op=ALU.subtract),
                                reads=["t1", "t2"], writes=[f"qk{qi}"])
                            P.op("dve", lambda e, xb=xb, Cb=Cb, t1v=t1v: e.tensor_tensor(out=t1v, in0=xb, in1=Cb, op=ALU.mult),
                                 reads=rk, writes=["t1"])
                            P.op("dve", lambda e, xa=xa, Sb=Sb, t2v=t2v: e.tensor_tensor(out=t2v, in0=xa, in1=Sb, op=ALU.mult),
                                 reads=rk, writes=["t2"])
                            P.op("dve", lambda e, dstv=dstv, t1v=t1v, t2v=t2v: e.tensor_tensor(
                                out=dstv[:, :, :, 1, :], in0=t1v, in1=t2v, op=ALU.add),
                                reads=["t1", "t2"], writes=[f"qk{qi}"])
                        else:
                            P.op("act", lambda e, b_=b_, qi=qi: e.copy(out=qk[:, qi, :], in_=pj[:, b_, :]),
                                 reads=[f"pj{b_}"], writes=[f"qk{qi}"])
                        for c in range(4):
                            P.op("pe", lambda e, c=c, qi=qi: e.transpose(out=pqT[:, qi, c, :], in_=qk[:, qi, c * 128:(c + 1) * 128],
                                                                        identity=identb[:]),
                                 reads=[f"qk{qi}", "identb"], writes=[f"pqT{qi}"])
                        P.op("act", lambda e, qi=qi: e.copy(out=qkT[:, qi], in_=pqT[:, qi, 0:4]),
                             reads=[f"pqT{qi}"], writes=[f"qkT{qi}"])
                        P.dma("sp", dst.rearrange("(c p) t -> p c t", p=128)[:, :, i * 128:(i + 1) * 128], qkT[:, qi],
                              reads=[f"qkT{qi}"], key=("qkT", qi))
                    b_ = proj(1024, 512)
                    P.op("act", lambda e, b_=b_, s_=s_: e.copy(out=vaug[:, s_, :, 0:128],
                                                                in_=pj[:, b_, :].rearrange("p (h v) -> p h v", h=4)),
                         reads=[f"pj{b_}"], writes=[f"vaug{s_}"])
                    P.dma("sp", VA[i * 128:(i + 1) * 128, :], vaug[:, s_].rearrange("p h v -> p (h v)"),
                          reads=[f"vaug{s_}"], key=("vaug", s_))
                    b_ = proj(1536, 512)
                    P.op("dve", lambda e, b_=b_, s_=s_: e.tensor_copy(out=btok[:, s_, 0:512], in_=pj[:, b_, :]),
                         reads=[f"pj{b_}"], writes=[f"btok{s_}"])
                    b_ = proj(2048, 256)
                    P.op("act", lambda e, b_=b_, s_=s_: e.copy(out=btok[:, s_, 512:768], in_=pj[:, b_, 0:256]),
                         reads=[f"pj{b_}"], writes=[f"btok{s_}"])
                    P.dma("sp", BTOK[i * 128:(i + 1) * 128, :], btok[:, s_, :], reads=[f"btok{s_}"], key=("btok", s_))
                    for kc in range(8):
                        P.op("pe", lambda e, kc=kc, s_=s_: e.matmul(pg[:, :], win[:, kc, 2304:2336], hT[:, s_, kc, :],
                                                                   start=(kc == 0), stop=(kc == 7)),
                             reads=[hTk] + winR, writes=["pg"])
                    P.op("dve", lambda e, s_=s_: e.tensor_copy(out=gbt[:, s_, :], in_=pg[:, :]), reads=["pg"], writes=[f"gbt{s_}"])
                    P.dma("sp", GBT[:, i * 128:(i + 1) * 128], gbt[:, s_, :], reads=[f"gbt{s_}"], key=("gbt", s_))
                    for c in range(4):
                        for kc in range(8):
                            P.op("pe", lambda e, kc=kc, c=c, s_=s_: e.matmul(
                                pn[:, c, :], win[:, kc, 2336 + c * 128:2336 + (c + 1) * 128], hT[:, s_, kc, :],
                                start=(kc == 0), stop=(kc == 7)),
                                reads=[hTk] + winR, writes=["pn"])
                    P.op("dve", lambda e, s_=s_: e.tensor_copy(out=nT[:, s_], in_=pn[:]), reads=["pn"], writes=[f"nT{s_}"])
                    P.dma("sp", QNT.rearrange("(c p) t -> p c t", p=128)[:, :, i * 128:(i + 1) * 128], nT[:, s_, 0:2, :],
                          reads=[f"nT{s_}"], key=("nTq", s_))
                    P.dma("sp", KNT.rearrange("(c p) t -> p c t", p=128)[:, :, i * 128:(i + 1) * 128], nT[:, s_, 2:4, :],
                          reads=[f"nT{s_}"], key=("nTk", s_))
                    b_ = proj(2848, 256)
                    P.op("act", lambda e, b_=b_, s_=s_: e.copy(out=vnaug[:, s_, :, 0:64],
                                                                in_=pj[:, b_, 0:256].rearrange("p (h v) -> p h v", h=4)),
                         reads=[f"pj{b_}"], writes=[f"vnaug{s_}"])
                    P.dma("sp", VN[i * 128:(i + 1) * 128, :], vnaug[:, s_].rearrange("p h v -> p (h v)"),
                          reads=[f"vnaug{s_}"], key=("vnaug", s_))

                for k_ in range(NT + 1):
                    if k_ < NT:
                        stage1(k_)
                    if k_ >= 1:
                        stage2(k_ - 1)
                P.end_phase()
                if stop_after == "proj" and l == n_layers - 1:
                    raise _Stop()

            with contextlib.ExitStack() as ph:
                def sb(name, shape, dt):
                    return ph.enter_context(nc.sbuf_tensor(f"{name}_a{l}", list(shape), dt))

                def pp(name, shape, dt):
                    return ph.enter_context(nc.psum_tensor(f"{name}_a{l}", list(shape), dt))

                kat = sb("kat", [128, 4, T], BF16)
                va = sb("va", [128, NT, 516], BF16)
                qt = sb("qt", [128, 2, 4, 512], BF16)
                Pm = sb("Pm", [128, 2, 2, 512], BF16)
                lamt = sb("lamt", [128, 4, 64], F32)
                lamp = sb("lamp", [128, 2, 64], F32)
                lams = sb("lams", [128, 2], F32)
                lamn = sb("lamn", [128, 1], F32)
                gsub = sb("gsub", [128, 128], F32)
                rr = sb("rr", [128, 2], F32)
                o0 = sb("o0", [128, 128], F32)
                o1 = sb("o1", [128, 128], F32)
                junk = sb("junk", [128, 128], F32)
                ss = sb("ss", [128, 1], F32)
                rstd = sb("rstd", [128, 1], F32)
                ya = sb("ya", [128, 2, 4, 512], BF16)
                Sps = pp("Sps", [128, 4, 512], F32)
                acc = pp("acc", [128, 4, 512], F32)

                def accv(m, sub):
                    a = m * 4 + sub
                    return acc[:, a // 2, (a % 2) * 129:(a % 2) * 129 + 129]

                katv = KAT.rearrange("(c p) t -> p c t", p=128)
                for c in range(4):
                    P.dma("sp", kat[:, c, :], katv[:, c, :], writes=[f"kat{c}"])
                vav = VA.rearrange("(n p) f -> p n f", p=128)
                for g_ in range(0, NT, 8):
                    g1_ = min(NT, g_ + 8)
                    P.dma("sp", va[:, g_:g1_, :], vav[:, g_:g1_, :], writes=["va"], key=("va", g_))
                vaR = ["va"]
                P.dma("sp", lamt[:].rearrange("p a d -> p (a d)"),
                      diff_lambda[l].rearrange("a d -> (a d)").partition_broadcast(128), writes=["lamt"])
                P.dma("sp", gsub[:], diff_sub_g[l].partition_broadcast(128), writes=["gsub"])
                P.op("dve", lambda e: e.tensor_tensor(out=lamp[:], in0=lamt[:].rearrange("p (a b) d -> p a b d", b=2)[:, :, 0, :],
                                                      in1=lamt[:].rearrange("p (a b) d -> p a b d", b=2)[:, :, 1, :], op=ALU.mult),
                     reads=["lamt"], writes=["lamp"])
                P.op("dve", lambda e: e.reduce_sum(out=lams[:], in_=lamp[:], axis=AX.X), reads=["lamp"], writes=["lams"])
                P.op("act", lambda e: e.activation(out=lams[:], in_=lams[:], func=AF.Exp), reads=["lams"], writes=["lams"])
                P.op("dve", lambda e: e.tensor_tensor(out=lamn[:], in0=lams[:, 1:2], in1=lams[:, 0:1], op=ALU.subtract),
                     reads=["lams"], writes=["lamn"])
                P.op("dve", lambda e: e.tensor_scalar_add(out=lamn[:], in0=lamn[:], scalar1=-lam_init), reads=["lamn"], writes=["lamn"])
                P.op("dve", lambda e: e.tensor_scalar_mul(out=gsub[:], in0=gsub[:], scalar1=(1.0 - lam_init)),
                     reads=["gsub"], writes=["gsub"])

                chunks = []
                if need_ctx:
                    chunks.append((0, 256, [0, 1]))
                for qc in range(8):
                    chunks.append((256 + qc * 512, 512, list(range(NT))))
                import os
                if os.environ.get("SKIP_A"):
                    chunks = []
                qatv = QAT.rearrange("(c p) t -> p c t", p=128)
                accS = sb("accS", [128, 2, 4, 258], F32)
                steps = []
                for qi, (tok0, nq, ktiles) in enumerate(chunks):
                    for h in range(4):
                        for ki, kt in enumerate(ktiles):
                            steps.append((qi, tok0, nq, h, kt, ki == 0, ki == len(ktiles) - 1,
                                          h == 0 and ki == 0, h == 3 and ki == len(ktiles) - 1))
                hcount = [0]

                def emit_S(k, st):
                    qi, tok0, nq, h, kt, first, last, cfirst, clast = st
                    qs = qi % 2
                    bsel = k % 2
                    if cfirst:
                        P.dma("sp", qt[:, qs, :, 0:nq], qatv[:, :, tok0:tok0 + nq], writes=[f"qt{qs}"])
                    for m in range(2):
                        P.op("pe", lambda e, m=m, h=h, kt=kt, bsel=bsel, qs=qs, nq=nq: e.matmul(
                            Sps[:, bsel * 2 + m, 0:nq], kat[m * 64:(m + 1) * 64, h, kt * 128:(kt + 1) * 128],
                            qt[m * 64:(m + 1) * 64, qs, h, 0:nq], start=True, stop=True),
                            reads=[f"kat{h}", f"qt{qs}"], writes=[f"S{bsel}{m}"])
                        P.op("act", lambda e, m=m, bsel=bsel, nq=nq: e.activation(
                            out=Pm[:, bsel, m, 0:nq], in_=Sps[:, bsel * 2 + m, 0:nq], func=AF.Exp, scale=0.125),
                            reads=[f"S{bsel}{m}"], writes=[f"P{bsel}{m}"])

                def emit_PV(k, st):
                    qi, tok0, nq, h, kt, first, last, cfirst, clast = st
                    qs = qi % 2
                    bsel = k % 2
                    nsub = nq // 128
                    for m in range(2):
                        for sub in range(nsub):
                            a = m * 4 + sub
                            P.op("pe", lambda e, m=m, sub=sub, h=h, kt=kt, bsel=bsel, st_=(first and a % 2 == 0), last=last: e.matmul(
                                accv(m, sub), Pm[:, bsel, m, sub * 128:(sub + 1) * 128], va[:, kt, h * 129:(h + 1) * 129],
                                start=st_, stop=last, skip_group_check=True),
                                reads=[f"P{bsel}{m}"] + vaR, writes=["acc"])
                    if not last:
                        return
                    hs = hcount[0] % 2
                    hcount[0] += 1
                    if nsub == 4:
                        P.op("dve", lambda e, hs=hs: e.tensor_copy(out=accS[:, hs], in_=acc[:, :, 0:258]), reads=["acc"], writes=[f"accS{hs}"])
                    else:
                        for bk in sorted({(m * 4 + sub) // 2 for m in range(2) for sub in range(nsub)}):
                            P.op("dve", lambda e, hs=hs, bk=bk: e.tensor_copy(out=accS[:, hs, bk, :], in_=acc[:, bk, 0:258]),
                                 reads=["acc"], writes=[f"accS{hs}"])
                    for sub in range(nsub):
                        def av_(m, sub=sub, hs=hs):
                            a = m * 4 + sub
                            return accS[:, hs, a // 2, (a % 2) * 129:(a % 2) * 129 + 129]
                        a0 = av_(0)
                        a1 = av_(1)
                        ak = f"accS{hs}"
                        P.op("dve", lambda e, a0=a0: e.reciprocal(out=rr[:, 0:1], in_=a0[:, 128:129]), reads=[ak], writes=["rr"])
                        P.op("dve", lambda e, a1=a1: e.reciprocal(out=rr[:, 1:2], in_=a1[:, 128:129]), reads=[ak], writes=["rr"])
                        P.op("dve", lambda e: e.tensor_tensor(out=rr[:, 1:2], in0=rr[:, 1:2], in1=lamn[:], op=ALU.mult),
                             reads=["rr", "lamn"], writes=["rr"])
                        P.op("dve", lambda e, a0=a0: e.tensor_scalar(out=o0[:], in0=a0[:, 0:128], scalar1=rr[:, 0:1], scalar2=None,
                                                                     op0=ALU.mult), reads=[ak, "rr"], writes=["o0"])
                        P.op("dve", lambda e, a1=a1: e.scalar_tensor_tensor(out=o1[:], in0=a1[:, 0:128], scalar=rr[:, 1:2], in1=o0[:],
                                                                            op0=ALU.mult, op1=ALU.add),
                             reads=[ak, "rr", "o0"], writes=["o1"])
                        P.op("dve", lambda e: e.tensor_tensor(out=junk[:], in0=o1[:], in1=o1[:], op=ALU.mult), reads=["o1"], writes=["junk"])
                        P.op("dve", lambda e: e.reduce_sum(out=ss[:], in_=junk[:], axis=AX.X), reads=["junk"], writes=["ss"])
                        P.op("act", lambda e: e.activation(out=rstd[:], in_=ss[:], func=AF.Ln, scale=1.0 / 128, bias=EPS),
                             reads=["ss"], writes=["rstd"])
                        P.op("act", lambda e: e.activation(out=rstd[:], in_=rstd[:], func=AF.Exp, scale=-0.5),
                             reads=["rstd"], writes=["rstd"])
                        P.op("dve", lambda e, sub=sub, h=h, qs=qs: e.scalar_tensor_tensor(
                            out=ya[:, qs, sub, h * 128:(h + 1) * 128], in0=o1[:], scalar=rstd[:], in1=gsub[:],
                            op0=ALU.mult, op1=ALU.mult), reads=["o1", "rstd", "gsub"], writes=[f"ya{qs}"])
                    if clast:
                        P.dma("sp", YCAT[tok0:tok0 + nq, 0:512].rearrange("(s p) f -> p s f", p=128), ya[:, qs, 0:nsub, :],
                              reads=[f"ya{qs}"], key=("ya", qs))

                for k in range(len(steps) + 1):
                    if k < len(steps):
                        emit_S(k, steps[k])
                    if k >= 1:
                        emit_PV(k - 1, steps[k - 1])
                P.end_phase()
                if stop_after == "A" and l == n_layers - 1:
                    raise _Stop()

            with contextlib.ExitStack() as ph:
                def sb(name, shape, dt):
                    return ph.enter_context(nc.sbuf_tensor(f"{name}_c{l}", list(shape), dt))

                def pp(name, shape, dt):
                    return ph.enter_context(nc.psum_tensor(f"{name}_c{l}", list(shape), dt))

                knt = sb("knt", [128, 2, T], BF16)
                qnt = sb("qnt", [128, 2, T], BF16)
                vn = sb("vn", [128, NT, 260], BF16)
                bm = sb("bm", [128, NPAT, 4, 128], F32)
                Tt = sb("Tt", [128, 2, 4, 128], F32)
                Pc = sb("Pc", [128, 2, 4, 128], BF16)
                rc = sb("rc", [128, 4], F32)
                yn = sb("yn", [128, 2, 4, 64], BF16)
                Sc = pp("Sc", [128, 2, 2, 512], F32)
                accC = pp("accC", [128, 2, 512], F32)

                for c in range(2):
                    P.dma("sp", knt[:, c, :], KNT.rearrange("(c p) t -> p c t", p=128)[:, c, :], writes=["knt"], key=("knt", c))
                    P.dma("sp", qnt[:, c, :], QNT.rearrange("(c p) t -> p c t", p=128)[:, c, :], writes=["qnt"], key=("qnt", c))
                vnv = VN.rearrange("(n p) f -> p n f", p=128)
                for g_ in range(0, NT, 8):
                    g1_ = min(NT, g_ + 8)
                    P.dma("sp", vn[:, g_:g1_, :], vnv[:, g_:g1_, :], writes=["vn"], key=("vn", g_))
                for p0 in range(0, NPAT, 4):
                    p1 = min(NPAT, p0 + 4)
                    P.dma("sp", bm[:, p0:p1], bmC_d[l, :, p0:p1], writes=["bm"], key=("bm", p0))
                cnt = 0
                import os
                _ct = os.environ.get("C_TILES")
                for i in (tiles_q if _ct is None else [int(v) for v in _ct.split(",")]):
                    if i >= 2:
                        keys = [(kt + 2, pid) for (kt, pid) in _NA_PLAN[i - 2]] + [(0, None), (1, None)]
                    else:
                        keys = [(0, None), (1, None)]
                    as_ = i % 2
                    _cm = int(os.environ.get("C_MODE", "9"))
                    if _cm == 0:
                        continue
                    av = accC[:, as_, 0:260].rearrange("p (h v) -> p h v", h=4)
                    P.op("dve", lambda e, as_=as_: e.memset(accC[:, as_, :], 0.0), writes=[f"accC{as_}"])
                    for ki, (kt, pid) in enumerate(keys):
                        b_ = cnt % 2
                        cnt += 1
                        for h in range(4):
                            hp, c = h % 2, h // 2
                            P.op("pe", lambda e, h=h, hp=hp, c=c, kt=kt, b_=b_, i=i: e.matmul(
                                Sc[:, b_, hp, c * 128:(c + 1) * 128], knt[hp * 64:(hp + 1) * 64, c, kt * 128:(kt + 1) * 128],
                                qnt[hp * 64:(hp + 1) * 64, c, i * 128:(i + 1) * 128], start=True, stop=True),
                                reads=["knt", "qnt"], writes=[f"Sc{b_}"])
                        if pid is not None:
                            P.op("dve", lambda e, b_=b_, pid=pid: e.scalar_tensor_tensor(
                                out=Tt[:, b_].rearrange("p (a b) q -> p a (b q)", a=2), in0=Sc[:, b_, :, 0:256], scalar=0.125,
                                in1=bm[:, pid].rearrange("p (a b) q -> p a (b q)", a=2), op0=ALU.mult, op1=ALU.add),
                                reads=[f"Sc{b_}", "bm"], writes=[f"Tt{b_}"])
                            P.op("act", lambda e, b_=b_: e.activation(out=Pc[:, b_], in_=Tt[:, b_], func=AF.Exp),
                                 reads=[f"Tt{b_}"], writes=[f"Pc{b_}"])
                        else:
                            P.op("act", lambda e, b_=b_: e.activation(out=Pc[:, b_].rearrange("p (a b) q -> p a (b q)", a=2),
                                                                      in_=Sc[:, b_, :, 0:256], func=AF.Exp, scale=0.125),
                                 reads=[f"Sc{b_}"], writes=[f"Pc{b_}"])
                        for h in range(4 if _cm >= 2 else 0):
                            P.op("pe", lambda e, h=h, kt=kt, b_=b_, av=av, last=(ki == len(keys) - 1): e.matmul(
                                av[:, h, :], Pc[:, b_, (h % 2) * 2 + h // 2, :], vn[:, kt, h * 65:(h + 1) * 65],
                                start=False, stop=last, skip_group_check=True),
                                reads=[f"Pc{b_}", "vn"], writes=[f"accC{as_}"])
                    if _cm < 3:
                        continue
                    P.op("dve", lambda e, av=av: e.reciprocal(out=rc[:], in_=av[:, :, 64]), reads=[f"accC{as_}"], writes=["rc"])
                    P.op("dve", lambda e, av=av, as_=as_: e.tensor_tensor(
                        out=yn[:, as_], in0=av[:, :, 0:64], in1=rc[:].unsqueeze(2).broadcast_to([128, 4, 64]), op=ALU.mult),
                        reads=[f"accC{as_}", "rc"], writes=[f"yn{as_}"])
                    P.dma("sp", YCAT[i * 128:(i + 1) * 128, 768:1024], yn[:, as_].rearrange("p h v -> p (h v)"),
                          reads=[f"yn{as_}"], key=("yn", as_))
                P.end_phase()
                if stop_after == "C" and l == n_layers - 1:
                    raise _Stop()

            with contextlib.ExitStack() as ph:
                def sb(name, shape, dt):
                    return ph.enter_context(nc.sbuf_tensor(f"{name}_b{l}", list(shape), dt))

                def pp(name, shape, dt):
                    return ph.enter_context(nc.psum_tensor(f"{name}_b{l}", list(shape), dt))

                ofs = sb("ofs", [128, NT, 256], F32)
                glt = sb("glt", [17, 2, T], F32)
                wdec = sb("wdec", [17, 2, 128], F32)
                glm = sb("glm", [128, 4, 128], F32)
                chunkind = sb("chunkind", [128, 2], F32)
                hm4 = sb("hm4", [128, 4, 128], BF16)
                bdm = sb("bdm", [128, 256], F32)
                gg = sb("gg", [128, 4, 64], F32)
                bt = sb("bt", [128, 2, 768], F32)
                vb = sb("vb", [128, 2, 256], BF16)
                e1 = sb("e1", [128, 128], F32)
                lap = sb("lap", [128, 128], F32)
                eb = sb("eb", [128, 3, 128], F32)
                gam = sb("gam", [128, 2], F32)
                qtl = sb("qtl", [128, 128], BF16)
                ktl = sb("ktl", [128, 128], BF16)
                kdec = sb("kdec", [128, 2, 128], BF16)
                qTs = sb("qTs", [128, 2, 128], BF16)
                kTs = sb("kTs", [128, 128], BF16)
                Qbd = sb("Qbd", [128, 4, 128], BF16)
                aM = sb("aM", [128, 4, 128], BF16)
                Sst = sb("Sst", [128, 256], F32)
                Sbf = sb("Sbf", [128, 4, 256], BF16)
                tmpi = sb("tmpi", [128, 256], F32)
                ob = sb("ob", [128, 256], F32)
                sq = sb("sq", [128, 256], F32)
                ssb = sb("ssb", [128, 4], F32)
                rsb = sb("rsb", [128, 4], F32)
                sr = sb("sr", [128, 256], F32)
                yb = sb("yb", [128, 2, 256], BF16)
                zps_full = pp("zps", [128, 512], F32)
                zps = zps_full[:, 0:128]
                cps = pp("cps", [128, 4, 128], F32)
                tps = pp("tps", [128, 8, 128], BF16)
                aps = pp("aps", [128, 512], F32)
                ips = pp("ips", [128, 2, 512], F32)
                ops_full = pp("ops", [128, 512], F32)
                ops_ = ops_full[:, 0:256]

                P.op("dve", lambda e: e.memset(glt[:], 1.0), writes=["glt"])
                for d_ in range(2):
                    P.dma("sp", glt[0:16, d_, :], GBT[d_ * 16:(d_ + 1) * 16, :], writes=["glt"], key=("glt", d_))
                    P.dma("sp", wdec[0:16, d_, :], gla_w_decay[l, d_], writes=["wdec"], key=("wdec", d_))
                    P.dma("sp", wdec[16:17, d_, :], gla_b_decay[l, d_:d_ + 1, :], writes=["wdec"], key=("wdecb", d_))
                P.dma("sp", glm[:], glm_d[:, :, :], writes=["glm"])
                P.dma("sp", chunkind[:], chunkind_d[:, :], writes=["chunkind"])
                P.dma("sp", hm4[:], hm4_d[:, :, :], writes=["hm4"])
                P.dma("sp", bdm[:], bd_d[:, :], writes=["bdm"])
                for h in range(4):
                    P.dma("sp", gg[:, h, :], gla_norm_g[l].partition_broadcast(128), writes=["gg"], key=("gg", h))

                scale_q = 32 ** -0.5
                for d_ in range(2):
                    if d_ == 0:
                        order = list(range(NT))
                        corder = (0, 1)
                    else:
                        order = [1, 0] + list(range(NT - 1, 1, -1))
                        corder = (1, 0)
                    Lm = glm[:, 2 * d_, :]
                    Dm = glm[:, 2 * d_ + 1, :]
                    P.op("dve", lambda e: e.memset(Sst[:], 0.0), writes=["Sst"])
                    P.op("dve", lambda e: e.memset(Sbf[:, 0, :], 0.0), writes=["Sbf0"])
                    step = 0
                    if os.environ.get("B_NT"):
                        order = order[:int(os.environ["B_NT"])]
                    if os.environ.get("B_DIR") and d_ != int(os.environ["B_DIR"]):
                        continue
                    for n_, i in enumerate(order):
                        s_ = n_ % 2
                        P.dma("sp", bt[:, s_, :], BTOK[i * 128:(i + 1) * 128, :], writes=[f"bt{s_}"])
                        P.op("act", lambda e, s_=s_: e.copy(out=vb[:, s_, :], in_=bt[:, s_, 256:512]), reads=[f"bt{s_}"], writes=[f"vb{s_}"])
                        P.op("pe", lambda e, i=i, d_=d_: e.matmul(zps[:], glt[:, d_, i * 128:(i + 1) * 128], wdec[:, d_, :],
                                                                 start=True, stop=True), reads=["glt", "wdec"], writes=["zps"])
                        P.op("act", lambda e: e.activation(out=e1[:], in_=zps[:], func=AF.Exp, scale=-1.0), reads=["zps"], writes=["e1"])
                        P.op("act", lambda e: e.activation(out=lap[:], in_=e1[:], func=AF.Ln, bias=1.0), reads=["e1"], writes=["lap"])
                        if int(os.environ.get("B_MODE", "9")) < 2:
                            continue
                        P.op("pe", lambda e, Lm=Lm: e.matmul(cps[:, 0, :], Lm, lap[:], start=True, stop=True),
                             reads=["glm", "lap"], writes=["cps"])
                        P.op("pe", lambda e, Dm=Dm: e.matmul(cps[:, 1, :], Dm, lap[:], start=True, stop=True),
                             reads=["glm", "lap"], writes=["cps"])
                        P.op("pe", lambda e: e.matmul(cps[:, 2, 0:2], lap[:], chunkind[:], start=True, stop=True),
                             reads=["chunkind", "lap"], writes=["cps"])
                        P.op("act", lambda e: e.activation(out=eb[:, 0, :], in_=cps[:, 0, :], func=AF.Exp, scale=-1.0 / 16),
                             reads=["cps"], writes=["eb0"])
                        P.op("act", lambda e: e.activation(out=eb[:, 1, :], in_=cps[:, 0, :], func=AF.Exp, scale=1.0 / 16),
                             reads=["cps"], writes=["eb1"])
                        P.op("act", lambda e: e.activation(out=eb[:, 2, :], in_=cps[:, 1, :], func=AF.Exp, scale=-1.0 / 16),
                             reads=["cps"], writes=["eb2"])
                        P.op("act", lambda e: e.activation(out=gam[:], in_=cps[:, 2, 0:2], func=AF.Exp, scale=-1.0 / 16),
                             reads=["cps"], writes=["gam"])
                        if int(os.environ.get("B_MODE", "9")) < 3:
                            continue
                        P.op("dve", lambda e, s_=s_: e.scalar_tensor_tensor(out=qtl[:], in0=bt[:, s_, 0:128], scalar=scale_q, in1=eb[:, 0, :],
                                                                          op0=ALU.mult, op1=ALU.mult),
                             reads=[f"bt{s_}", "eb0"], writes=["qtl"])
                        P.op("dve", lambda e, s_=s_: e.tensor_tensor(out=ktl[:], in0=bt[:, s_, 128:256], in1=eb[:, 1, :], op=ALU.mult),
                             reads=[f"bt{s_}", "eb1"], writes=["ktl"])
                        P.op("dve", lambda e, s_=s_: e.tensor_tensor(out=kdec[:, s_, :], in0=bt[:, s_, 128:256], in1=eb[:, 2, :], op=ALU.mult),
                             reads=[f"bt{s_}", "eb2"], writes=[f"kdec{s_}"])
                        if int(os.environ.get("B_SUB", "9")) < 2:
                            continue
                        P.op("pe", lambda e: e.transpose(out=tps[:, 0, :], in_=qtl[:], identity=identb[:]),
                             reads=["qtl", "identb"], writes=["tps"])
                        P.op("pe", lambda e: e.transpose(out=tps[:, 1, :], in_=ktl[:], identity=identb[:]),
                             reads=["ktl", "identb"], writes=["tps"])
                        P.op("act", lambda e, s_=s_: e.copy(out=qTs[:, s_, :], in_=tps[:, 0, :]), reads=["tps"], writes=[f"qTs{s_}"])
                        if int(os.environ.get("B_SUB", "9")) < 3:
                            continue
                        P.op("dve", lambda e, s_=s_: e.tensor_tensor(out=Qbd[:], in0=hm4[:],
                                                                     in1=qTs[:, s_, :].unsqueeze(1).broadcast_to([128, 4, 128]), op=ALU.mult),
                             reads=[f"qTs{s_}", "hm4"], writes=["Qbd"])
                        P.op("act", lambda e: e.copy(out=kTs[:], in_=tps[:, 1, :]), reads=["tps"], writes=["kTs"])
                        P.op("pe", lambda e: e.matmul(aps[:], kTs[:], Qbd[:].rearrange("p h t -> p (h t)"), start=True, stop=True),
                             reads=["kTs", "Qbd"], writes=["aps"])
                        if int(os.environ.get("B_SUB", "9")) < 4:
                            continue
                        P.op("dve", lambda e, Lm=Lm: e.tensor_tensor(out=aM[:], in0=aps[:].rearrange("p (h t) -> p h t", h=4),
                                                                     in1=Lm.unsqueeze(1).broadcast_to([128, 4, 128]), op=ALU.mult),
                             reads=["aps", "glm"], writes=["aM"])
                        if int(os.environ.get("B_MODE", "9")) < 4:
                            continue
                        for c in corder:
                            P.op("pe", lambda e, c=c, s_=s_: e.matmul(ips[:, c, 0:256], kdec[c * 64:(c + 1) * 64, s_, :], vb[c * 64:(c + 1) * 64, s_, :],
                                                                     start=True, stop=True),
                                 reads=[f"kdec{s_}", f"vb{s_}"], writes=[f"ips{c}"])
                        for h in range(4):
                            P.op("pe", lambda e, h=h, s_=s_: e.matmul(ops_[:, h * 64:(h + 1) * 64], aM[:, h, :], vb[:, s_, h * 64:(h + 1) * 64],
                                                                     start=(h == 0), stop=False, skip_group_check=True),
                                 reads=["aM", f"vb{s_}"], writes=["ops"])
                        if int(os.environ.get("B_MODE", "9")) < 5:
                            continue
                        for ci, c in enumerate(corder):
                            st = step % 4
                            P.op("pe", lambda e, c=c, st=st, s_=s_, ci=ci: e.matmul(
                                ops_[c * 64:(c + 1) * 64, :], qTs[:, s_, c * 64:(c + 1) * 64], Sbf[:, st, :],
                                start=False, stop=(ci == 1), skip_group_check=True),
                                reads=[f"qTs{s_}", f"Sbf{st}"], writes=["ops"])
                            P.op("dve", lambda e, c=c: e.tensor_tensor(out=tmpi[:], in0=ips[:, c, 0:256], in1=bdm[:], op=ALU.mult),
                                 reads=[f"ips{c}", "bdm"], writes=["tmpi"])
                            P.op("dve", lambda e, c=c: e.scalar_tensor_tensor(out=Sst[:], in0=Sst[:], scalar=gam[:, c:c + 1], in1=tmpi[:],
                                                                              op0=ALU.mult, op1=ALU.add),
                                 reads=["Sst", "gam", "tmpi"], writes=["Sst"])
                            step += 1
                            st2 = step % 4
                            P.op("dve", lambda e, st2=st2: e.tensor_copy(out=Sbf[:, st2, :], in_=Sst[:]), reads=["Sst"], writes=[f"Sbf{st2}"])
                        if d_ == 0:
                            P.op("dve", lambda e, i=i: e.tensor_copy(out=ofs[:, i, :], in_=ops_[:]), reads=["ops"], writes=[f"ofs{i}"])
                        else:
                            P.op("dve", lambda e, i=i: e.tensor_tensor(out=ob[:], in0=ops_[:], in1=ofs[:, i, :], op=ALU.add),
                                 reads=["ops", f"ofs{i}"], writes=["ob"])
                            P.op("dve", lambda e: e.tensor_tensor(out=sq[:], in0=ob[:], in1=ob[:], op=ALU.mult), reads=["ob"], writes=["sq"])
                            P.op("dve", lambda e: e.reduce_sum(out=ssb[:], in_=sq[:].rearrange("p (h v) -> p h v", h=4), axis=AX.X),
                                 reads=["sq"], writes=["ssb"])
                            P.op("act", lambda e: e.activation(out=rsb[:], in_=ssb[:], func=AF.Ln, scale=1.0 / 64, bias=EPS),
                                 reads=["ssb"], writes=["rsb"])
                            P.op("act", lambda e: e.activation(out=rsb[:], in_=rsb[:], func=AF.Exp, scale=-0.5), reads=["rsb"], writes=["rsb"])
                            P.op("act", lambda e, s_=s_: e.activation(out=sr[:], in_=bt[:, s_, 512:768], func=AF.Silu),
                                 reads=[f"bt{s_}"], writes=["sr"])
                            obv = ob[:].rearrange("p (h v) -> p h v", h=4)
                            P.op("dve", lambda e, obv=obv: e.tensor_tensor(out=obv, in0=obv, in1=rsb[:].unsqueeze(2).broadcast_to([128, 4, 64]),
                                                                           op=ALU.mult), reads=["ob", "rsb"], writes=["ob"])
                            P.op("dve", lambda e, obv=obv: e.tensor_tensor(out=obv, in0=obv, in1=gg[:], op=ALU.mult),
                                 reads=["ob", "gg"], writes=["ob"])
                            P.op("dve", lambda e, s_=s_: e.tensor_tensor(out=yb[:, s_, :], in0=ob[:], in1=sr[:], op=ALU.mult),
                                 reads=["ob", "sr"], writes=[f"yb{s_}"])
                            P.dma("sp", YCAT[i * 128:(i + 1) * 128, 512:768], yb[:, s_, :], reads=[f"yb{s_}"], key=("yb", s_))
                P.end_phase()
                if stop_after == "B" and l == n_layers - 1:
                    raise _Stop()

            with contextlib.ExitStack() as ph:
                def sb(name, shape, dt):
                    return ph.enter_context(nc.sbuf_tensor(f"{name}_o{l}", list(shape), dt))

                def pp(name, shape, dt):
                    return ph.enter_context(nc.psum_tensor(f"{name}_o{l}", list(shape), dt))

                wo = sb("wo", [128, 8, D], BF16)
                wr = sb("wr", [128, 8, 36], BF16)
                br = sb("br", [1, 36], BF16)
                G1 = sb("G1", [128, 2, D], F32)
                A2 = sb("A2", [128, 2, D], F32)
                S2 = sb("S2", [128, 2, D], F32)
                yc = sb("yc", [128, 2, D], BF16)
                ycT = sb("ycT", [128, 8, 128], BF16)
                xt = sb("xt", [128, 2, D], F32)
                xn = sb("xn", [128, 2, D], F32)
                tmp = sb("tmp", [128, D], F32)
                junk = sb("junk", [128, D], F32)
                ss = sb("ss", [128, 1], F32)
                rstd = sb("rstd", [128, 1], F32)
                h2b = sb("h2b", [128, D], BF16)
                h2T = sb("h2T", [128, 2, 8, 128], BF16)
                lg = sb("lg", [128, 36], F32)
                sm = sb("sm", [128, 16], F32)
                goh = sb("goh", [128, 4], F32)
                eg = sb("eg", [128, 4], F32)
                lm = sb("lm", [128, 32], F32)
                lm2 = sb("lm2", [128, 32], F32)
                oh1 = sb("oh1", [128, 32], F32)
                oh2 = sb("oh2", [128, 32], F32)
                gt1 = sb("gt1", [128, 32], F32)
                pT = pp("pT", [128, 8, 128], BF16)
                pd = pp("pd", [128, 2, 512], F32)
                pT2 = pp("pT2", [128, 8, 128], BF16)
                pl_full = pp("pl", [128, 512], F32)
                pl = pl_full[:, 0:36]

                for kc in range(8):
                    P.dma("pool", wo[:, kc, :], w_out[l, kc * 128:(kc + 1) * 128, :], writes=["wo"], key=("wo", kc))
                P.dma("pool", wr[:, :, 0:4], w_rg[l].rearrange("(k p) n -> p k n", p=128), writes=["wr"], key=("wr", 0))
                P.dma("pool", wr[:, :, 4:36], w_re[l].rearrange("(k p) n -> p k n", p=128), writes=["wr"], key=("wr", 1))
                P.dma("pool", br[:, 0:4], b_rg[l:l + 1, :], writes=["br"], key=("br", 0))
                P.dma("pool", br[:, 4:36], b_re[l:l + 1, :], writes=["br"], key=("br", 1))
                for t_ in range(2):
                    P.dma("sp", G1[:, t_, :], modrow[t_, 2].partition_broadcast(128), writes=["G1"], key=("G1", t_))
                    P.dma("sp", A2[:, t_, :], modrow[t_, 4].partition_broadcast(128), writes=["A2"], key=("A2", t_))
                    P.dma("sp", S2[:, t_, :], modrow[t_, 3].partition_broadcast(128), writes=["S2"], key=("S2", t_))

                for i in tiles_q:
                    s_ = i % 2
                    lat = 0 if i >= 2 else 1
                    P.dma("sp", yc[:, s_, :], YCAT[i * 128:(i + 1) * 128, :], writes=[f"yc{s_}"])
                    P.dma("sp", xt[:, s_, :], x_rows(l, i), writes=[f"xt{s_}"])
                    for kc in range(8):
                        P.op("pe", lambda e, kc=kc, s_=s_: e.transpose(out=pT[:, kc, :], in_=yc[:, s_, kc * 128:(kc + 1) * 128],
                                                                      identity=identb[:]),
                             reads=[f"yc{s_}", "identb"], writes=["pT"])
                    P.op("act", lambda e: e.copy(out=ycT[:], in_=pT[:]), reads=["pT"], writes=["ycT"])
                    for half in range(2):
                        for kc in range(8):
                            P.op("pe", lambda e, kc=kc, half=half: e.matmul(pd[:, half, :], ycT[:, kc, :], wo[:, kc, half * 512:(half + 1) * 512],
                                                                           start=(kc == 0), stop=(kc == 7)),
                                 reads=["ycT", "wo"], writes=[f"pd{half}"])
                        P.op("dve", lambda e, half=half, lat=lat: e.tensor_tensor(
                            out=tmp[:, half * 512:(half + 1) * 512], in0=pd[:, half, :], in1=G1[:, lat, half * 512:(half + 1) * 512], op=ALU.mult),
                            reads=[f"pd{half}", "G1"], writes=["tmp"])
                    P.op("dve", lambda e, s_=s_: e.tensor_tensor(out=xn[:, s_, :], in0=tmp[:], in1=xt[:, s_, :], op=ALU.add),
                         reads=["tmp", f"xt{s_}"], writes=[f"xn{s_}"])
                    P.dma("sp", xs[i * 128:(i + 1) * 128, :], xn[:, s_, :], reads=[f"xn{s_}"], key=("xn", s_))
                    P.op("act", lambda e, s_=s_: e.activation(out=junk[:], in_=xn[:, s_, :], func=AF.Square, accum_out=ss[:]),
                         reads=[f"xn{s_}"], writes=["junk", "ss"])
                    P.op("act", lambda e: e.activation(out=rstd[:], in_=ss[:], func=AF.Ln, scale=1.0 / D, bias=EPS), reads=["ss"], writes=["rstd"])
                    P.op("act", lambda e: e.activation(out=rstd[:], in_=rstd[:], func=AF.Exp, scale=-0.5), reads=["rstd"], writes=["rstd"])
                    P.op("dve", lambda e, s_=s_, lat=lat: e.scalar_tensor_tensor(out=tmp[:], in0=xn[:, s_, :], scalar=rstd[:], in1=A2[:, lat, :],
                                                                                op0=ALU.mult, op1=ALU.mult),
                         reads=[f"xn{s_}", "rstd", "A2"], writes=["tmp"])
                    P.op("dve", lambda e, lat=lat: e.tensor_tensor(out=h2b[:], in0=tmp[:], in1=S2[:, lat, :], op=ALU.add),
                         reads=["tmp", "S2"], writes=["h2b"])
                    for kc in range(8):
                        P.op("pe", lambda e, kc=kc: e.transpose(out=pT2[:, kc, :], in_=h2b[:, kc * 128:(kc + 1) * 128], identity=identb[:]),
                             reads=["h2b", "identb"], writes=["pT2"])
                    P.op("act", lambda e, s_=s_: e.copy(out=h2T[:, s_], in_=pT2[:]), reads=["pT2"], writes=[f"h2T{s_}"])
                    P.dma("sp", H2T.rearrange("(c p) t -> p c t", p=128)[:, :, i * 128:(i + 1) * 128], h2T[:, s_],
                          reads=[f"h2T{s_}"], key=("h2T", s_))
                    for kc in range(8):
                        P.op("pe", lambda e, kc=kc, s_=s_: e.matmul(pl[:], h2T[:, s_, kc, :], wr[:, kc, :], start=(kc == 0), stop=False),
                             reads=[f"h2T{s_}", "wr"], writes=["pl"])
                    P.op("pe", lambda e: e.matmul(pl[:], ones_b[:], br[:], start=False, stop=True), reads=["ones_b", "br"], writes=["pl"])
                    P.op("dve", lambda e: e.tensor_copy(out=lg[:], in_=pl[:]), reads=["pl"], writes=["lg"])
                    P.op("dve", lambda e: e.reduce_max(out=sm[:, 0:1], in_=lg[:, 0:4], axis=AX.X), reads=["lg"], writes=["sm"])
                    P.op("dve", lambda e: e.tensor_scalar(out=goh[:], in0=lg[:, 0:4], scalar1=sm[:, 0:1], scalar2=None, op0=ALU.is_equal),
                         reads=["lg", "sm"], writes=["goh"])
                    P.op("dve", lambda e: e.tensor_scalar_mul(out=sm[:, 1:2], in0=sm[:, 0:1], scalar1=-1.0), reads=["sm"], writes=["sm"])
                    P.op("act", lambda e: e.activation(out=eg[:], in_=lg[:, 0:4], func=AF.Exp, bias=sm[:, 1:2], accum_out=sm[:, 2:3]),
                         reads=["lg", "sm"], writes=["eg", "sm"])
                    P.op("dve", lambda e: e.tensor_scalar(out=goh[:], in0=goh[:], scalar1=1e4, scalar2=-1e4, op0=ALU.mult, op1=ALU.add),
                         reads=["goh"], writes=["goh"])
                    P.op("dve", lambda e: e.tensor_tensor(out=lm[:].rearrange("p (g k) -> p g k", g=4),
                                                          in0=lg[:, 4:36].rearrange("p (g k) -> p g k", g=4),
                                                          in1=goh[:].unsqueeze(2).broadcast_to([128, 4, 8]), op=ALU.add),
                         reads=["lg", "goh"], writes=["lm"])
                    P.op("dve", lambda e: e.reduce_max(out=sm[:, 3:4], in_=lm[:], axis=AX.X), reads=["lm"], writes=["sm"])
                    P.op("dve", lambda e: e.tensor_scalar(out=oh1[:], in0=lm[:], scalar1=sm[:, 3:4], scalar2=None, op0=ALU.is_equal),
                         reads=["lm", "sm"], writes=["oh1"])
                    P.op("dve", lambda e: e.scalar_tensor_tensor(out=lm2[:], in0=oh1[:], scalar=-1e4, in1=lm[:], op0=ALU.mult, op1=ALU.add),
                         reads=["oh1", "lm"], writes=["lm2"])
                    P.op("dve", lambda e: e.reduce_max(out=sm[:, 4:5], in_=lm2[:], axis=AX.X), reads=["lm2"], writes=["sm"])
                    P.op("dve", lambda e: e.tensor_scalar(out=oh2[:], in0=lm2[:], scalar1=sm[:, 4:5], scalar2=None, op0=ALU.is_equal),
                         reads=["lm2", "sm"], writes=["oh2"])
                    P.op("dve", lambda e: e.tensor_tensor(out=sm[:, 5:6], in0=sm[:, 4:5], in1=sm[:, 3:4], op=ALU.subtract), reads=["sm"], writes=["sm"])
                    P.op("act", lambda e: e.activation(out=sm[:, 6:7], in_=sm[:, 5:6], func=AF.Exp), reads=["sm"], writes=["sm"])
                    P.op("dve", lambda e: e.scalar_tensor_tensor(out=sm[:, 7:8], in0=sm[:, 6:7], scalar=1.0, in1=sm[:, 2:3], op0=ALU.add, op1=ALU.mult),
                         reads=["sm"], writes=["sm"])
                    P.op("dve", lambda e: e.reciprocal(out=sm[:, 8:9], in_=sm[:, 7:8]), reads=["sm"], writes=["sm"])
                    P.op("dve", lambda e: e.tensor_tensor(out=sm[:, 9:10], in0=sm[:, 8:9], in1=sm[:, 6:7], op=ALU.mult), reads=["sm"], writes=["sm"])
                    P.op("dve", lambda e: e.tensor_scalar(out=gt1[:], in0=oh1[:], scalar1=sm[:, 8:9], scalar2=None, op0=ALU.mult),
                         reads=["oh1", "sm"], writes=["gt1"])
                    P.op("dve", lambda e, i=i: e.scalar_tensor_tensor(out=G_all[:, i, :], in0=oh2[:], scalar=sm[:, 9:10], in1=gt1[:],
                                                                     op0=ALU.mult, op1=ALU.add),
                         reads=["oh2", "sm", "gt1"], writes=[f"G{i}"])
                P.end_phase()
                if stop_after == "out" and l == n_layers - 1:
                    raise _Stop()

            halves = [list(range(0, 17)), list(range(17, NT))]
            for hf, htiles in enumerate(halves):
                htiles = [i for i in htiles if i in tiles_q]
                with contextlib.ExitStack() as ph:
                    def sb(name, shape, dt):
                        return ph.enter_context(nc.sbuf_tensor(f"{name}_e{l}_{hf}", list(shape), dt))

                    def pp(name, shape, dt):
                        return ph.enter_context(nc.psum_tensor(f"{name}_e{l}_{hf}", list(shape), dt))

                    nth = len(htiles)
                    t0 = htiles[0] * 128
                    yacc = sb("yacc", [128, nth, D], F32)
                    h2t = sb("h2t", [128, 8, nth * 128], BF16)
                    wu = sb("wu", [128, 2, 8, D], BF16)
                    wd = sb("wd", [128, 2, 4, D], BF16)
                    sg = sb("sg", [128, 2, 512], F32)
                    aj = sb("aj", [128, 2, 4, 512], BF16)
                    G2 = sb("G2", [128, 2, D], F32)
                    xt = sb("xt", [128, 2, D], F32)
                    xf = sb("xf", [128, 2, D], F32)
                    fg = sb("fg", [128, D], F32)
                    junk = sb("junk", [128, D], F32)
                    ss = sb("ss", [128, 1], F32)
                    rstd = sb("rstd", [128, 1], F32)
                    pu = pp("pu", [128, 4, 512], F32)
                    pdn = pp("pdn", [128, 2, 512], F32)

                    h2v = H2T.rearrange("(c p) t -> p c t", p=128)
                    for kc in range(8):
                        P.dma("sp", h2t[:, kc, :], h2v[:, kc, t0:t0 + nth * 128], writes=["h2t"], key=("h2t", kc))
                    for t_ in range(2):
                        P.dma("sp", G2[:, t_, :], modrow[t_, 5].partition_broadcast(128), writes=["G2"], key=("G2", t_))
                    if last_layer:
                        P.dma("sp", fg[:], final_g.partition_broadcast(128), writes=["fg"])
                    tchunks = []
                    p_ = 0
                    while p_ < nth:
                        n_ = min(4, nth - p_)
                        tchunks.append((p_, n_))
                        p_ += n_
                    cnts = {"u": 0, "d": 0}
                    msteps = [(ex, tp, ntl) for ex in range(NE) for (tp, ntl) in tchunks]

                    def up_stage(k):
                        ex, tp, ntl = msteps[k]
                        ws = ex % 2
                        if tp == 0:
                            for kc in range(8):
                                P.dma("pool", wu[:, ws, kc, :], w_up[l, ex, kc * 128:(kc + 1) * 128, :], writes=[f"wu{ws}"], key=("wu", ws, kc))
                            for jc in range(4):
                                P.dma("pool", wd[:, ws, jc, :], w_down[l, ex, jc * 128:(jc + 1) * 128, :], writes=[f"wd{ws}"], key=("wd", ws, jc))
                        N = ntl * 128
                        as_ = k % 2
                        for j in range(4):
                            ub = cnts["u"] % 2
                            cnts["u"] += 1
                            for gu in range(2):
                                oc = j + 4 * gu
                                for kc in range(8):
                                    P.op("pe", lambda e, kc=kc, oc=oc, ub=ub, gu=gu, ws=ws, tp=tp, N=N: e.matmul(
                                        pu[:, ub * 2 + gu, 0:N], wu[:, ws, kc, oc * 128:(oc + 1) * 128], h2t[:, kc, tp * 128:tp * 128 + N],
                                        start=(kc == 0), stop=(kc == 7)),
                                        reads=[f"wu{ws}", "h2t"], writes=[f"pu{ub}{gu}"])
                            P.op("act", lambda e, ub=ub, N=N: e.activation(out=sg[:, ub, 0:N], in_=pu[:, ub * 2, 0:N], func=AF.Silu),
                                 reads=[f"pu{ub}0"], writes=[f"sg{ub}"])
                            P.op("dve", lambda e, ub=ub, N=N, as_=as_, j=j: e.tensor_tensor(
                                out=aj[:, as_, j, 0:N], in0=pu[:, ub * 2 + 1, 0:N], in1=sg[:, ub, 0:N], op=ALU.mult),
                                reads=[f"pu{ub}1", f"sg{ub}"], writes=[f"aj{as_}"])

                    def down_stage(k):
                        ex, tp, ntl = msteps[k]
                        ws = ex % 2
                        as_ = k % 2
                        for sub in range(ntl):
                            ti = tp + sub
                            gi = htiles[ti]
                            for half in range(2):
                                db = cnts["d"] % 2
                                cnts["d"] += 1
                                for j in range(4):
                                    P.op("pe", lambda e, j=j, db=db, as_=as_, sub=sub, ws=ws, half=half: e.matmul(
                                        pdn[:, db, :], aj[:, as_, j, sub * 128:(sub + 1) * 128], wd[:, ws, j, half * 512:(half + 1) * 512],
                                        start=(j == 0), stop=(j == 3)),
                                        reads=[f"aj{as_}", f"wd{ws}"], writes=[f"pdn{db}"])
                                yv = yacc[:, ti, half * 512:(half + 1) * 512]
                                if ex == 0:
                                    P.op("dve", lambda e, db=db, yv=yv, gi=gi, ex=ex: e.tensor_scalar(
                                        out=yv, in0=pdn[:, db, :], scalar1=G_all[:, gi, ex:ex + 1], scalar2=None, op0=ALU.mult),
                                        reads=[f"pdn{db}", f"G{gi}"], writes=[f"yacc{ti}_{half}"])
                                else:
                                    P.op("dve", lambda e, db=db, yv=yv, gi=gi, ex=ex: e.scalar_tensor_tensor(
                                        out=yv, in0=pdn[:, db, :], scalar=G_all[:, gi, ex:ex + 1], in1=yv, op0=ALU.mult, op1=ALU.add),
                                        reads=[f"pdn{db}", f"G{gi}", f"yacc{ti}_{half}"], writes=[f"yacc{ti}_{half}"])

                    for k_ in range(len(msteps) + 1):
                        if k_ < len(msteps):
                            up_stage(k_)
                        if k_ >= 1:
                            down_stage(k_ - 1)
                    for ti, gi in enumerate(htiles):
                        s_ = ti % 2
                        lat = 0 if gi >= 2 else 1
                        P.dma("sp", xt[:, s_, :], xs[gi * 128:(gi + 1) * 128, :], writes=[f"xt{s_}"])
                        P.op("dve", lambda e, ti=ti, lat=lat: e.tensor_tensor(out=yacc[:, ti, :], in0=yacc[:, ti, :], in1=G2[:, lat, :], op=ALU.mult),
                             reads=[f"yacc{ti}_0", f"yacc{ti}_1", "G2"], writes=[f"yacc{ti}_0", f"yacc{ti}_1"])
                        P.op("dve", lambda e, ti=ti, s_=s_: e.tensor_tensor(out=xf[:, s_, :], in0=yacc[:, ti, :], in1=xt[:, s_, :], op=ALU.add),
                             reads=[f"yacc{ti}_0", f"yacc{ti}_1", f"xt{s_}"], writes=[f"xf{s_}"])
                        if not last_layer:
                            P.dma("sp", xs[gi * 128:(gi + 1) * 128, :], xf[:, s_, :], reads=[f"xf{s_}"], key=("xf", s_))
                        else:
                            P.op("act", lambda e, s_=s_: e.activation(out=junk[:], in_=xf[:, s_, :], func=AF.Square, accum_out=ss[:]),
                                 reads=[f"xf{s_}"], writes=["junk", "ss"])
                            P.op("act", lambda e: e.activation(out=rstd[:], in_=ss[:], func=AF.Ln, scale=1.0 / D, bias=EPS),
                                 reads=["ss"], writes=["rstd"])
                            P.op("act", lambda e: e.activation(out=rstd[:], in_=rstd[:], func=AF.Exp, scale=-0.5), reads=["rstd"], writes=["rstd"])
                            P.op("dve", lambda e, s_=s_: e.scalar_tensor_tensor(out=xf[:, s_, :], in0=xf[:, s_, :], scalar=rstd[:], in1=fg[:],
                                                                                op0=ALU.mult, op1=ALU.mult),
                                 reads=[f"xf{s_}", "rstd", "fg"], writes=[f"xf{s_}"])
                            P.dma("sp", out_d[(gi - 2) * 128:(gi - 1) * 128, :], xf[:, s_, :], reads=[f"xf{s_}"], key=("xf", s_))
                    P.end_phase()
    P.close()
    return nc


_CONSTS = None


def make_in_maps(inputs, n_layers=DEPTH, ne_decl=None):
    global _CONSTS
    if _CONSTS is None:
        _CONSTS = host_consts()
    f = lambda a: np.ascontiguousarray(np.asarray(a, dtype=np.float32))
    fl = lambda a: np.ascontiguousarray(np.asarray(a, dtype=np.float32)[:n_layers])
    shared = {k: (f(inputs[k]) if k in ("c_ctx", "final_g") else fl(inputs[k])) for k in ("c_ctx", "w_mod", "b_mod", "norm1_g", "norm2_g", "w_in", "w_out", "diff_lambda",
                                        "diff_sub_g", "gla_w_decay", "gla_b_decay", "gla_norm_g", "w_router_group",
                                        "b_router_group", "w_router_expert", "b_router_expert", "w_expert_up",
                                        "w_expert_down", "final_g")}
    shared.update(_CONSTS)
    if ne_decl is not None:
        shared["w_expert_up"] = np.ascontiguousarray(shared["w_expert_up"][:, :ne_decl])
        shared["w_expert_down"] = np.ascontiguousarray(shared["w_expert_down"][:, :ne_decl])
    shared["bmC"] = na_bias_host(fl(inputs["na_rel_bias"]))
    x = f(inputs["x"])
    c = f(inputs["c"])
    ctx = f(inputs["ctx"])
    maps = []
    for b in range(8):
        m = dict(shared)
        m["x"] = x[b]
        m["c"] = c[b]
        m["ctx"] = ctx[b]
        maps.append(m)
    return maps


def kernel(**inputs):
    nc = build_program()
    maps = make_in_maps(inputs)
    res = run_bass_kernel_spmd(nc, maps, core_ids=list(range(8)))
    out = np.stack([np.asarray(res.results[b]["out"], dtype=np.float32) for b in range(8)], axis=0)
    return out
```
